# Optimizing a Trainium2 kernel written in Bass

```python
import math
import jax, jax.numpy as jnp
from jax import lax
import numpy as np

D_MODEL = 1024
BATCH = 32
SEQ = 2048
DEPTH = 1

MEM_LEN = 256
D_MIX = D_MODEL
HEAD_DIM = 64
ATTN_HEADS = 8
D_ATTN = ATTN_HEADS * HEAD_DIM
CONV_GROUPS = 8
D_CONV = D_MIX - D_ATTN
CONV_WIDTH = 3
MOBA_BLOCK = 256
MOBA_TOPK = 3
Q_CHUNK = 128
MEM_HEADS = 4
MEM_HEAD_DIM = D_MODEL // MEM_HEADS
D_FF = ((8 * D_MODEL // 3) + 127) // 128 * 128
FFN_CONV_WIDTH = 3
EPS = 1e-6
D_IN_MIX = 3 * D_ATTN + 3 * D_CONV

kernel_name = "hymba_moba_shortconv_convffn_layer"


def rmsnorm(x, g):
    xf = x.astype(jnp.float32)
    r = lax.rsqrt(jnp.mean(xf * xf, axis=-1, keepdims=True) + EPS)
    return (xf * r).astype(x.dtype) * g


def causal_dwconv(x, w):
    K = w.shape[0]
    S = x.shape[1]
    xp = jnp.pad(x, ((0, 0), (K - 1, 0), (0, 0)))
    y = xp[:, 0:S] * w[0]
    for k in range(1, K):
        y = y + xp[:, k:k + S] * w[k]
    return y


def moba_attention(q, k, v):
    B, S, H, Dh = q.shape
    L = MOBA_BLOCK
    nb = -(-S // L)
    pad = nb * L - S
    topk = max(1, min(MOBA_TOPK, nb - 1))
    scale = Dh ** -0.5
    kp = jnp.pad(k, ((0, 0), (0, pad), (0, 0), (0, 0)))
    vp = jnp.pad(v, ((0, 0), (0, pad), (0, 0), (0, 0)))
    kb = kp.reshape(B, nb, L, H, Dh).transpose(0, 3, 1, 2, 4)
    vb = vp.reshape(B, nb, L, H, Dh).transpose(0, 3, 1, 2, 4)
    kmean = jnp.mean(kb.astype(jnp.float32), axis=3)

    pos = jnp.arange(S)
    qblk = pos // L
    gate = jnp.einsum('bshd,bhnd->bhsn', q.astype(jnp.float32), kmean)
    past = jnp.arange(nb)[None, :] < qblk[:, None]
    gate = jnp.where(past[None, None], gate, -jnp.inf)
    _, sel = lax.top_k(gate, topk)

    nc = S // Q_CHUNK
    qc = q.reshape(B, nc, Q_CHUNK, H, Dh).transpose(0, 1, 3, 2, 4).reshape(B * nc, H, Q_CHUNK, Dh)
    selc = sel.reshape(B, H, nc, Q_CHUNK, topk).transpose(0, 2, 1, 3, 4).reshape(B * nc, H, Q_CHUNK, topk)

    def one_chunk(args):
        i, q_c, sel_c = args
        b = i // nc
        c = i % nc
        k_b = kb[b]
        v_b = vb[b]
        qpos = c * Q_CHUNK + jnp.arange(Q_CHUNK)
        own = (c * Q_CHUNK) // L
        k_sel = jax.vmap(lambda kh, sh: kh[sh])(k_b, sel_c)
        v_sel = jax.vmap(lambda vh, sh: vh[sh])(v_b, sel_c)
        s_sel = jnp.einsum('hcd,hcjld->hcjl', q_c, k_sel).astype(jnp.float32) * scale
        ok_sel = jnp.arange(topk)[None, :] < (qpos // L)[:, None]
        s_sel = jnp.where(ok_sel[None, :, :, None], s_sel, -jnp.inf)
        k_own = lax.dynamic_index_in_dim(k_b, own, axis=1, keepdims=False)
        v_own = lax.dynamic_index_in_dim(v_b, own, axis=1, keepdims=False)
        s_own = jnp.einsum('hcd,hld->hcl', q_c, k_own).astype(jnp.float32) * scale
        kpos = own * L + jnp.arange(L)
        s_own = jnp.where((kpos[None, :] <= qpos[:, None])[None], s_own, -jnp.inf)
        s = jnp.concatenate([s_sel.reshape(H, Q_CHUNK, topk * L), s_own], axis=-1)
        p = jax.nn.softmax(s, axis=-1).astype(v_b.dtype)
        p_sel = p[..., :topk * L].reshape(H, Q_CHUNK, topk, L)
        p_own = p[..., topk * L:]
        o = jnp.einsum('hcjl,hcjld->hcd', p_sel, v_sel) + jnp.einsum('hcl,hld->hcd', p_own, v_own)
        return o

    out = lax.map(one_chunk, (jnp.arange(B * nc, dtype=jnp.int32), qc, selc))
    return out.reshape(B, nc, H, Q_CHUNK, Dh).transpose(0, 1, 3, 2, 4).reshape(B, S, H, Dh)


def parallel_mixer(h, w_in, conv_w, attn_g, conv_g, w_out):
    B, S, _ = h.shape
    proj = h @ w_in
    o1 = D_ATTN
    o2 = 2 * D_ATTN
    o3 = 3 * D_ATTN
    o4 = o3 + D_CONV
    o5 = o4 + D_CONV
    q, k, v, bg, cg, u = jnp.split(proj, [o1, o2, o3, o4, o5], axis=-1)
    q = q.reshape(B, S, ATTN_HEADS, HEAD_DIM)
    k = k.reshape(B, S, ATTN_HEADS, HEAD_DIM)
    v = v.reshape(B, S, ATTN_HEADS, HEAD_DIM)
    y_attn = moba_attention(q, k, v).reshape(B, S, D_ATTN)
    y_conv = bg * causal_dwconv(cg * u, conv_w)
    y = jnp.concatenate([rmsnorm(y_attn, attn_g), rmsnorm(y_conv, conv_g)], axis=-1)
    return y @ w_out


def memory_cross_attention(h, m, w_q, w_kv, w_o):
    B, S, _ = h.shape
    M = m.shape[1]
    q = (h @ w_q).reshape(B, S, MEM_HEADS, MEM_HEAD_DIM)
    kv = m @ w_kv
    k = kv[..., :D_MODEL].reshape(B, M, MEM_HEADS, MEM_HEAD_DIM)
    v = kv[..., D_MODEL:].reshape(B, M, MEM_HEADS, MEM_HEAD_DIM)
    s = jnp.einsum('bshd,bmhd->bhsm', q, k).astype(jnp.float32) * (MEM_HEAD_DIM ** -0.5)
    p = jax.nn.softmax(s, axis=-1).astype(v.dtype)
    o = jnp.einsum('bhsm,bmhd->bshd', p, v).reshape(B, S, D_MODEL)
    return o @ w_o


def conv_gated_mlp(h, w_up, conv_w, w_down):
    up = h @ w_up
    g = causal_dwconv(up[..., :D_FF], conv_w)
    u = up[..., D_FF:]
    return (jax.nn.silu(g) * u) @ w_down


def setup_inputs(seed: int = 0) -> dict:
    key = jax.random.key(seed)
    ks = jax.random.split(key, 20)
    f32 = jnp.float32

    def w(k, shape, fan_in):
        return jax.random.normal(k, shape, f32) * (fan_in ** -0.5)

    def gain(k, shape):
        return 1.0 + 0.1 * jax.random.normal(k, shape, f32)

    return {
        "x": jax.random.normal(ks[0], (BATCH, SEQ, D_MODEL), f32),
        "mem": jax.random.normal(ks[1], (BATCH, MEM_LEN, D_MODEL), f32),
        "norm_mix_g": gain(ks[2], (DEPTH, D_MODEL)),
        "w_in_mix": w(ks[3], (DEPTH, D_MODEL, D_IN_MIX), D_MODEL),
        "conv_mix_w": w(ks[4], (DEPTH, CONV_WIDTH, D_CONV), CONV_WIDTH),
        "attn_out_g": gain(ks[5], (DEPTH, D_ATTN)),
        "conv_out_g": gain(ks[6], (DEPTH, D_CONV)),
        "w_out_mix": w(ks[7], (DEPTH, D_MIX, D_MODEL), D_MIX),
        "norm_mem_g": gain(ks[8], (DEPTH, D_MODEL)),
        "mem_kv_norm_g": gain(ks[9], (DEPTH, D_MODEL)),
        "w_mem_q": w(ks[10], (DEPTH, D_MODEL, D_MODEL), D_MODEL),
        "w_mem_kv": w(ks[11], (DEPTH, D_MODEL, 2 * D_MODEL), D_MODEL),
        "w_mem_o": w(ks[12], (DEPTH, D_MODEL, D_MODEL), D_MODEL),
        "norm_ffn_g": gain(ks[13], (DEPTH, D_MODEL)),
        "w_ffn_up": w(ks[14], (DEPTH, D_MODEL, 2 * D_FF), D_MODEL),
        "ffn_conv_w": w(ks[15], (DEPTH, FFN_CONV_WIDTH, D_FF), FFN_CONV_WIDTH),
        "w_ffn_down": w(ks[16], (DEPTH, D_FF, D_MODEL), D_FF),
        "final_norm_g": gain(ks[17], (D_MODEL,)),
    }


def reference(x, mem, norm_mix_g, w_in_mix, conv_mix_w, attn_out_g, conv_out_g,
              w_out_mix, norm_mem_g, mem_kv_norm_g, w_mem_q, w_mem_kv, w_mem_o,
              norm_ffn_g, w_ffn_up, ffn_conv_w, w_ffn_down, final_norm_g):
    for l in range(DEPTH):
        h = rmsnorm(x, norm_mix_g[l])
        x = x + parallel_mixer(h, w_in_mix[l], conv_mix_w[l], attn_out_g[l],
                               conv_out_g[l], w_out_mix[l])
        h = rmsnorm(x, norm_mem_g[l])
        m = rmsnorm(mem, mem_kv_norm_g[l])
        x = x + memory_cross_attention(h, m, w_mem_q[l], w_mem_kv[l], w_mem_o[l])
        h = rmsnorm(x, norm_ffn_g[l])
        x = x + conv_gated_mlp(h, w_ffn_up[l], ffn_conv_w[l], w_ffn_down[l])
    return rmsnorm(x, final_norm_g)
```

```python
import numpy as np
from contextlib import ExitStack
import concourse.bass as bass
import concourse.mybir as mybir
from concourse.bass_utils import run_bass_kernel_spmd

F32 = mybir.dt.float32
BF16 = mybir.dt.bfloat16
AF = mybir.ActivationFunctionType
ALU = mybir.AluOpType
AX = mybir.AxisListType

D = 1024
KC = 8
S_LEN = 2048
TT = 4
NT = 16
MEM = 256
DFF = 2816
NJ = 22
EPS = 1e-6
GROUPS = [(0, 6), (6, 6), (12, 5), (17, 5)]
NPAR = 128
NEG = -30000.0


class Buf:
    __slots__ = ("name", "w", "r")

    def __init__(self, name):
        self.name = name
        self.w = {}
        self.r = {}


class Sched:
    ENG = ("pe", "act", "dve", "pool", "sp")

    def __init__(self, nc, es):
        self.nc = nc
        self.es = es
        self.streams = {e: [] for e in self.ENG}
        self.cnt = {}
        self.seen = {e: {} for e in self.ENG}
        self.sems = {}
        for e in ("pe", "act", "dve", "pool"):
            self._sem("E_" + e)

    def _sem(self, key):
        if key not in self.sems:
            self.sems[key] = self.es.enter_context(self.nc.semaphore("s_" + key))
            self.cnt[key] = 0
        return self.sems[key]

    def _deps(self, eng, reads, writes):
        need = {}
        own = "E_" + eng

        def add(d, war=False):
            for k, v in d.items():
                if k == own and eng == "pe":
                    continue
                if need.get(k, 0) < v:
                    need[k] = v
        for b in reads:
            add(b.w)
        for b in writes:
            add(b.w)
            add(b.r, war=True)
        out = []
        seen = self.seen[eng]
        for k, v in need.items():
            if seen.get(k, 0) < v:
                seen[k] = v
                out.append((k, v))
        return out

    def _mark(self, key, val, reads, writes):
        for b in reads:
            if b.r.get(key, 0) < val:
                b.r[key] = val
        for b in writes:
            b.w = {key: val}
            b.r = {}

    def op(self, eng, insts, reads=(), writes=()):
        waits = self._deps(eng, reads, writes)
        key = "E_" + eng
        self.cnt[key] += 1
        val = self.cnt[key]
        self.streams[eng].append((waits, insts, key, 1))
        self._mark(key, val, reads, writes)

    def dma(self, q, out_ap, in_ap, semkey, reads=(), writes=()):
        self._sem(semkey)
        waits = self._deps(q, reads, writes)
        self.cnt[semkey] += 16
        val = self.cnt[semkey]
        self.streams[q].append((waits, [("dma_start", (), dict(out=out_ap, in_=in_ap))], semkey, 16))
        self._mark(semkey, val, reads, writes)

    def barrier(self, engs=("pe", "act", "dve", "sp")):
        for e in engs:
            waits = []
            for k, v in self.cnt.items():
                if (k == "E_" + e and e == "pe") or v == 0:
                    continue
                if self.seen[e].get(k, 0) < v:
                    self.seen[e][k] = v
                    waits.append((k, v))
            if waits:
                self.streams[e].append((waits, [], None, 0))

    def final_wait(self, eng="sp"):
        waits = [(k, v) for k, v in self.cnt.items() if v > 0 and k != "E_" + eng]
        self.streams[eng].append((waits, [], None, 0))

    def check_deadlock(self):
        sem = {k: 0 for k in self.cnt}
        pos = {e: 0 for e in self.ENG}
        prog = True
        while prog:
            prog = False
            for e in self.ENG:
                st = self.streams[e]
                while pos[e] < len(st):
                    waits, insts, key, inc = st[pos[e]]
                    if any(sem[k] < v for k, v in waits):
                        break
                    if insts and key is not None:
                        sem[key] += inc
                    pos[e] += 1
                    prog = True
        stuck = {e: (pos[e], len(self.streams[e])) for e in self.ENG if pos[e] < len(self.streams[e])}
        for e in stuck:
            waits, insts, key, inc = self.streams[e][pos[e]]
            print("STUCK", e, pos[e], [(k, v, sem[k]) for k, v in waits if sem[k] < v], [i[0] for i in insts][:2])
        return not stuck

    def emit(self, block):
        hooks = {"pe": block.tensor, "act": block.scalar, "dve": block.vector,
                 "pool": block.gpsimd, "sp": block.sync}
        for eng in self.ENG:
            stream = self.streams[eng]
            if not stream:
                continue

            def body(e, stream=stream):
                for waits, insts, key, inc in stream:
                    for k, v in waits:
                        e.wait_ge(self.sems[k], v)
                    ins = None
                    for m, a, kw in insts:
                        ins = getattr(e, m)(*a, **kw)
                    if ins is not None and key is not None:
                        ins.then_inc(self.sems[key], inc)
            hooks[eng](body)


class Rot:
    def __init__(self, items):
        self.items = items
        self.i = 0

    def next(self):
        x = self.items[self.i % len(self.items)]
        self.i += 1
        return x


def build_nc(NS, debug=False):
    nc = bass.Bass("TRN2", target_bir_lowering=False)
    dt = lambda n, s, k="ExternalInput": nc.dram_tensor(n, s, F32, kind=k).ap()
    x_d = dt("x", [NS, 128, KC, S_LEN])
    mem_d = dt("mem", [NS, 128, KC, MEM])
    win_d = dt("w_in", [128, KC, 3072])
    wout_d = dt("w_out", [128, KC, 1024])
    wmq_d = dt("w_mq", [128, KC, 1024])
    wmkv_d = dt("w_mkv", [128, KC, 2048])
    wmo_d = dt("w_mo", [128, KC, 1024])
    wup_d = dt("w_up", [128, KC, 2 * DFF])
    wdn_d = dt("w_dn", [128, NJ, 1024])
    par_d = dt("par", [128, NPAR])
    garow_d = dt("garow", [1, 512])
    cst_d = dt("cst", [128, 320])
    out_d = dt("out", [NS, 128, KC, S_LEN], "ExternalOutput")
    if debug:
        dbg_d = dt("dbg", [3, 128, KC, S_LEN], "ExternalOutput")

    es = ExitStack()
    with es:
        S = Sched(nc, es)
        sb = lambda n, s, d=F32: es.enter_context(nc.sbuf_tensor(n, s, d))
        XT = sb("XT", [128, KC, S_LEN])
        HT = sb("HT", [128, KC, S_LEN], BF16)
        SCR = sb("SCR", [128, 10248])
        scrb = SCR[:, :].bitcast(BF16)
        YTA = scrb[:, 0:8192].rearrange("p (a b) -> p a b", a=4)
        RING = sb("RING", [128, 6, 2048], BF16)
        PAR = sb("PAR", [128, NPAR])
        GA = sb("GA", [128, 512])
        CST = sb("CST", [128, 320])
        IDB = sb("IDB", [128, 128], BF16)
        TRI = sb("TRI", [128, 128], BF16)
        ONES = sb("ONES", [128, 128], BF16)
        EPST = sb("EPST", [128, 1])
        SQ = [sb(f"SQ{i}", [128, 512], BF16) for i in range(3)]
        RT = [sb(f"RT{i}", [128, 512]) for i in range(2)]
        TMP = [sb(f"TMP{i}", [128, 512]) for i in range(4)]
        YC = [SCR[:, 4096 + i * 512:4096 + (i + 1) * 512] for i in range(4)]
        CU = [SCR[:, 6144 + i * 514:6144 + (i + 1) * 514] for i in range(4)]
        PT = [sb(f"PT{i}", [128, 512], BF16) for i in range(4)]
        ACC = [sb(f"ACC{i}", [128, 65]) for i in range(4)]
        SM = [sb(f"SM{i}", [128, 4]) for i in range(4)]
        GS = [sb(f"GS{i}", [128, 8, 8]) for i in range(2)]
        TOP = [sb(f"TOP{i}", [128, 8, 8]) for i in range(2)]
        KMS = sb("KMS", [128, 4, 8])
        KMB = sb("KMB", [128, 4, 8], BF16)
        YN = [sb(f"YN{i}", [128, 512], BF16) for i in range(2)]
        MT = SCR[:, 8192:10240].rearrange("p (a b) -> p a b", a=KC)
        MTB = sb("MTB", [128, KC, MEM], BF16)
        MKT = sb("MKT", [128, KC, MEM], BF16)
        MV = sb("MV", [128, 2, 1024], BF16)
        G = [SCR[:, 6144 + i * 2050:6144 + (i + 1) * 2050] for i in range(2)]
        AT = scrb[:, 0:12288].rearrange("p (a b) -> p a b", a=6)
        xflat = XT[:, :, :].rearrange("p a b -> p (a b)")
        xbf = xflat.bitcast(BF16) if hasattr(xflat, "bitcast") else None
        assert xbf is not None
        KT = xbf[:, 0:8192].rearrange("p (a b) -> p a b", a=4)
        QT = xbf[:, 8192:16384].rearrange("p (a b) -> p a b", a=4)
        VAF = xbf[:, 16384:16384 + 16 * 8 * 66]
        VA = xbf[:, 16384:16384 + 16 * 8 * 66].rearrange("p (a b c) -> p a b c", a=16, b=8)
        mo = (16384 + 16 * 8 * 66) // 2
        MASK = xflat[:, mo:mo + 1024].rearrange("p (a b c) -> p a b c", a=16, b=8)
        QMT = scrb[:, 0:16384].rearrange("p (a b) -> p a b", a=KC)
        pst = lambda n, s, d=F32: es.enter_context(nc.psum_tensor(n, s, d))
        PB = [pst(f"PB{i}", [128, 512]) for i in range(8)]
        bPB = [Buf(f"PB{i}") for i in range(8)]
        B = lambda n: Buf(n)
        bXT = [[B(f"XT{o}_{t}") for t in range(TT)] for o in range(KC)]
        bHT = [[B(f"HT{o}_{t}") for t in range(TT)] for o in range(KC)]
        bYTA = [[B(f"YTA{o}_{t}") for t in range(TT)] for o in range(4)]
        bRING = [B(f"RING{i}") for i in range(6)]
        bPAR, bGA, bCST, bIDB, bTRI, bONES, bEPS = B("PAR"), B("GA"), B("CST"), B("IDB"), B("TRI"), B("ONES"), B("EPS")
        rSQ = Rot([(SQ[i], B(f"SQ{i}")) for i in range(3)])
        rRT = Rot([(RT[i], B(f"RT{i}")) for i in range(2)])
        rTMP = Rot([(TMP[i], B(f"TMP{i}")) for i in range(4)])
        bYC = [B(f"YC{i}") for i in range(4)]
        bCU = [B(f"CU{i}") for i in range(4)]
        rPT = Rot([(PT[i], B(f"PT{i}")) for i in range(4)])
        rACC = Rot([(ACC[i], B(f"ACC{i}")) for i in range(4)])
        rSM = Rot([(SM[i], B(f"SM{i}")) for i in range(4)])
        rGS = Rot([(GS[i], TOP[i], B(f"GS{i}"), B(f"TOP{i}")) for i in range(2)])
        bKMS, bKMB = B("KMS"), B("KMB")
        rYN = Rot([(YN[i], B(f"YN{i}")) for i in range(2)])
        bMT, bMTB, bMKT, bMV = B("MT"), B("MTB"), B("MKT"), B("MV")
        rG = Rot([(G[i], B(f"G{i}")) for i in range(2)])
        bAT = [[B(f"AT{j}_{t}") for t in range(TT)] for j in range(6)]
        bKT = [[B(f"KT{p}_{t}") for t in range(TT)] for p in range(4)]
        bQT = [[B(f"QT{p}_{t}") for t in range(TT)] for p in range(4)]
        bVA = [[B(f"VA{i}_{u}") for u in range(2)] for i in range(NT)]
        bMASK = [B(f"MASK{i}") for i in range(NT)]
        bQMT = [[B(f"QMT{o}_{t}") for t in range(TT)] for o in range(KC)]
        rPMM = Rot([(PB[i], bPB[i]) for i in range(4)])
        rPTR = Rot([(PB[i][:, :].bitcast(BF16)[:, 0:512], bPB[i]) for i in (6, 7)])
        rPSS = Rot([(PB[i], bPB[i]) for i in (4, 5)])
        rPO = Rot([(PB[i][:, 0:65], bPB[i]) for i in (0, 1, 2, 3, 6, 7)])

        tsl = lambda t: slice(t * 512, (t + 1) * 512)

        units = []

        def add_units(w_d, c0, ncols, rows=KC, r0=0, step=256):
            for c in range(c0, c0 + ncols, step):
                units.append((w_d[:, r0:r0 + rows, c:c + step], rows, step))
        for s in range(NS):
            add_units(win_d, 512, 512)
            add_units(win_d, 1024, 512)
            add_units(win_d, 0, 512)
            add_units(win_d, 1536, 1536)
            add_units(wout_d, 0, 1024)
            add_units(wmq_d, 0, 1024)
            add_units(wmkv_d, 0, 2048)
            add_units(wmo_d, 0, 1024)
            for (j0, nj) in GROUPS:
                add_units(wup_d, j0 * 256, nj * 256)
                add_units(wdn_d, 0, 1024, rows=nj, r0=j0, step=128)
        wstate = {"issued": 0, "next": 0}
        LOOK = 5

        def issue_loads(upto):
            while wstate["issued"] < min(upto, len(units)):
                i = wstate["issued"]
                ap, rows, cols = units[i]
                sl = i % 6
                dst = RING[:, sl, 0:rows * cols].rearrange("p (r c) -> p r c", r=rows)
                S.dma("pool", dst, ap, f"ring{sl}", writes=[bRING[sl]])
                wstate["issued"] += 1

        def consume(cap=None):
            i = wstate["next"]
            wstate["next"] += 1
            issue_loads(i + LOOK + 1 if cap is None else min(i + LOOK + 1, cap))
            ap, rows, cols = units[i]
            sl = i % 6
            return RING[:, sl, 0:rows * cols].rearrange("p (r c) -> p r c", r=rows), bRING[sl]

        def mm(out_ap, out_b, pairs, reads):
            n = len(pairs)
            insts = [("matmul", (out_ap, l, r), dict(start=(i == 0), stop=(i == n - 1)))
                     for i, (l, r) in enumerate(pairs)]
            S.op("pe", insts, reads=reads, writes=[out_b])

        def act(out_ap, in_ap, func, reads, writes, **kw):
            S.op("act", [("activation", (), dict(out=out_ap, in_=in_ap, func=func, **kw))], reads=reads, writes=writes)

        def dve(method, reads, writes, **kw):
            S.op("dve", [(method, (), kw)], reads=reads, writes=writes)

        def rstd_from_ps(ps_ap, ps_b, n, inv_d):
            rt, rb = rRT.next()
            act(rt[:, 0:n], ps_ap, AF.Sqrt, [ps_b, bEPS], [rb], scale=inv_d, bias=EPST[:, 0:1])
            dve("reciprocal", [rb], [rb], out=rt[:, 0:n], in_=rt[:, 0:n])
            return rt[:, 0:n], rb

        def norm_stage(gcol, dst_fn):
            for tt in range(TT):
                ps, pb = rPMM.next()
                for kc in range(KC):
                    sq, sqb = rSQ.next()
                    act(sq[:], XT[:, kc, tsl(tt)], AF.Square, [bXT[kc][tt]], [sqb])
                    S.op("pe", [("matmul", (ps[:], ONES[:], sq[:]), dict(start=(kc == 0), stop=(kc == KC - 1)))],
                         reads=[sqb, bONES], writes=[pb])
                r, rb = rstd_from_ps(ps[:], pb, 512, 1.0 / D)
                for kc in range(KC):
                    o_ap, o_b = dst_fn(kc, tt)
                    dve("scalar_tensor_tensor", [bXT[kc][tt], rb, bPAR], [o_b], out=o_ap, in0=XT[:, kc, tsl(tt)],
                        scalar=PAR[:, gcol + kc:gcol + kc + 1], in1=r, op0=ALU.mult, op1=ALU.mult)

        def load_x(s):
            for tt in range(TT):
                S.dma("sp", XT[:, :, tsl(tt)], x_d[s, :, :, tsl(tt)], f"xld{tt}", writes=[bXT[k][tt] for k in range(KC)])

        def proj_accum(w_d_units, src, src_b, dst_add=True):
            for u in range(4):
                W, wb = consume()
                for ocl in range(2):
                    oc = 2 * u + ocl
                    for tt in range(TT):
                        ps, pb = rPMM.next()
                        mm(ps[:], pb, [(W[:, kc, ocl * 128:(ocl + 1) * 128], src(kc, tt)) for kc in range(KC)],
                           [wb] + [src_b(kc, tt) for kc in range(KC)])
                        dve("tensor_tensor", [pb, bXT[oc][tt]], [bXT[oc][tt]], out=XT[:, oc, tsl(tt)], in0=ps[:],
                            in1=XT[:, oc, tsl(tt)], op=ALU.add)

        def dump(idx):
            if not debug:
                return
            S.barrier(("sp",))
            for tt in range(TT):
                S.dma("sp", dbg_d[idx, :, :, tsl(tt)], XT[:, :, tsl(tt)], f"dbg{tt}", reads=[bXT[k][tt] for k in range(KC)])

        S.dma("sp", PAR[:], par_d, "ldpar", writes=[bPAR])
        S.dma("sp", GA[:], garow_d.to_broadcast([128, 512]), "ldga", writes=[bGA])
        S.dma("sp", CST[:], cst_d, "ldcst", writes=[bCST])
        dve("tensor_copy", [bCST], [bIDB], out=IDB[:], in_=CST[:, 0:128])
        dve("tensor_copy", [bCST], [bTRI], out=TRI[:], in_=CST[:, 128:256])
        dve("memset", [], [bONES], ap=ONES[:], constant=1.0)
        dve("memset", [], [bEPS], ap=EPST[:], constant=EPS)
        NEGM = CST[:, 256:320].rearrange("p (a b) -> p a b", a=8)

        for s in range(NS):
            load_x(s)
            norm_stage(0, lambda kc, tt: (HT[:, kc, tsl(tt)], bHT[kc][tt]))
            S.barrier()
            dve("memset", [], [b for i in range(NT) for b in bVA[i]], ap=VAF, constant=1.0)
            for c in range(4):
                dve("memset", [], [bCU[c]], ap=CU[c][:, 0:2], constant=0.0)
            for u in range(2):
                W, wb = consume()
                for pcl in range(2):
                    pc = 2 * u + pcl
                    for tt in range(TT):
                        ps, pb = rPMM.next()
                        mm(ps[:], pb, [(W[:, kc, pcl * 128:(pcl + 1) * 128], HT[:, kc, tsl(tt)]) for kc in range(KC)],
                           [wb] + [bHT[kc][tt] for kc in range(KC)])
                        act(KT[:, pc, tsl(tt)], ps[:], AF.Copy, [pb], [bKT[pc][tt]])
            for u in range(2):
                W, wb = consume()
                for i in range(NT):
                    ps, pb = rPMM.next()
                    tt, o = i // 4, (i % 4) * 128
                    mm(ps[:, 0:256], pb, [(HT[:, kc, tt * 512 + o:tt * 512 + o + 128], W[:, kc, 0:256]) for kc in range(KC)],
                       [wb] + [bHT[kc][tt] for kc in range(KC)])
                    dve("tensor_copy", [pb], [bVA[i][u]], out=VA[:, i, 4 * u:4 * u + 4, 0:64],
                        in_=ps[:, 0:256].rearrange("p (h d) -> p h d", h=4))
            for u in range(2):
                W, wb = consume()
                for pcl in range(2):
                    pc = 2 * u + pcl
                    for tt in range(TT):
                        ps, pb = rPMM.next()
                        mm(ps[:], pb, [(W[:, kc, pcl * 128:(pcl + 1) * 128], HT[:, kc, tsl(tt)]) for kc in range(KC)],
                           [wb] + [bHT[kc][tt] for kc in range(KC)])
                        act(QT[:, pc, tsl(tt)], ps[:], AF.Copy, [pb], [bQT[pc][tt]])
            dve("tensor_reduce", [bKT[p][t] for p in range(4) for t in range(TT)], [bKMS],
                out=KMS[:, :, :].rearrange("p a b -> p (a b)"),
                in_=KT.rearrange("p a (n l) -> p (a n) l", l=256), axis=AX.X, op=ALU.add)
            act(KMB[:], KMS[:], AF.Copy, [bKMS], [bKMB], scale=1.0 / 256.0)
            for i in range(NT):
                qb = i // 2
                tt = i // 4
                ps, pb = rPMM.next()
                insts = []
                for h in range(8):
                    pc, hp = h // 2, h % 2
                    insts.append(("matmul", (ps[:, h * 8:h * 8 + 8], QT[hp * 64:hp * 64 + 64, pc, i * 128:(i + 1) * 128],
                                             KMB[hp * 64:hp * 64 + 64, pc, :]), dict(start=True, stop=True)))
                S.op("pe", insts, reads=[bKMB] + [bQT[p][tt] for p in range(4)], writes=[pb])
                gs, top, gsb, topb = rGS.next()
                dve("tensor_tensor", [pb, bCST], [gsb], out=gs[:], in0=ps[:, 0:64].rearrange("p (h n) -> p h n", h=8),
                    in1=NEGM[:, qb:qb + 1, :].to_broadcast([128, 8, 8]), op=ALU.add)
                S.op("dve", [("max", (), dict(out=top[:, h, :], in_=gs[:, h, :])) for h in range(8)], reads=[gsb], writes=[topb])
                dve("tensor_tensor", [gsb, topb], [bMASK[i]], out=MASK[:, i, :, :], in0=gs[:],
                    in1=top[:, :, 3:4].to_broadcast([128, 8, 8]), op=ALU.is_ge)
            cap = wstate["next"] + 6
            Wc = [consume(cap) for _ in range(6)]

            def wblk(c, which):
                blk = c * 3 + which
                W, wb = Wc[blk // 2]
                return W, wb, (blk % 2) * 128
            for tt in range(TT):
                pss, pssb = rPSS.next()
                for c in range(4):
                    pp = []
                    for which in range(3):
                        W, wb, co = wblk(c, which)
                        ps, pb = rPMM.next()
                        mm(ps[:], pb, [(W[:, kc, co:co + 128], HT[:, kc, tsl(tt)]) for kc in range(KC)],
                           [wb] + [bHT[kc][tt] for kc in range(KC)])
                        pp.append((ps, pb))
                        if which == 1:
                            cs, csb = rTMP.next()
                            act(cs[:], ps[:], AF.Copy, [pb], [csb])
                    (pB, pBb), (pC, pCb), (pU, pUb) = pp
                    dve("tensor_tensor", [pUb, csb], [bCU[c]], out=CU[c][:, 2:514], in0=pU[:], in1=cs[:], op=ALU.mult)
                    t, tb = rTMP.next()
                    cw = 44 + c * 3
                    act(t[:], CU[c][:, 2:514], AF.Copy, [bCU[c], bPAR], [tb], scale=PAR[:, cw + 2:cw + 3])
                    dve("scalar_tensor_tensor", [bCU[c], tb, bPAR], [tb], out=t[:], in0=CU[c][:, 1:513],
                        scalar=PAR[:, cw + 1:cw + 2], in1=t[:], op0=ALU.mult, op1=ALU.add)
                    dve("scalar_tensor_tensor", [bCU[c], tb, bPAR], [tb], out=t[:], in0=CU[c][:, 0:512],
                        scalar=PAR[:, cw:cw + 1], in1=t[:], op0=ALU.mult, op1=ALU.add)
                    dve("tensor_tensor", [pBb, tb], [bYC[c]], out=YC[c][:], in0=pB[:], in1=t[:], op=ALU.mult)
                    dve("tensor_copy", [bCU[c]], [bCU[c]], out=CU[c][:, 0:2], in_=CU[c][:, 512:514])
                    sq, sqb = rSQ.next()
                    act(sq[:], YC[c][:], AF.Square, [bYC[c]], [sqb])
                    S.op("pe", [("matmul", (pss[:], ONES[:], sq[:]), dict(start=(c == 0), stop=(c == 3)))],
                         reads=[sqb, bONES], writes=[pssb])
                r, rb = rstd_from_ps(pss[:], pssb, 512, 1.0 / 512.0)
                for c in range(4):
                    dve("scalar_tensor_tensor", [bYC[c], rb, bPAR], [bHT[c][tt]], out=HT[:, c, tsl(tt)], in0=YC[c][:],
                        scalar=PAR[:, 40 + c:41 + c], in1=r, op0=ALU.mult, op1=ALU.mult)
            pass
            S.barrier()
            YA = HT[:, 4:8, :].rearrange("p a (t f) -> p (a t) f", f=512)
            bYA = [B(f"YA{s}_{i}") for i in range(NT)]
            for h in range(8):
                pc, hp = h // 2, h % 2
                ksl = slice(hp * 64, hp * 64 + 64)
                for qb in range(8):
                    q0 = qb * 256
                    tq = qb // 2
                    accs = [rACC.next() for _ in range(2)]
                    for n in range(qb + 1):
                        own = (n == qb)
                        tk = n // 2
                        sp, spb = rPSS.next()
                        pt, ptb = rPT.next()
                        rd = [bKT[pc][tk], bQT[pc][tq]]
                        if not own:
                            insts = [("matmul", (sp[:, j * 256:(j + 1) * 256], KT[ksl, pc, (2 * n + j) * 128:(2 * n + j + 1) * 128],
                                                 QT[ksl, pc, q0:q0 + 256]), dict(start=True, stop=True)) for j in range(2)]
                            S.op("pe", insts, reads=rd, writes=[spb])
                            act(pt[:], sp[:], AF.Exp, [spb], [ptb], scale=0.125)
                        else:
                            k0 = 2 * n * 128
                            insts = [
                                ("matmul", (sp[:, 128:256], KT[ksl, pc, k0:k0 + 128], QT[ksl, pc, q0 + 128:q0 + 256]), dict(start=True, stop=True)),
                                ("matmul", (sp[:, 0:128], KT[ksl, pc, k0:k0 + 128], QT[ksl, pc, q0:q0 + 128]), dict(start=True, stop=False)),
                                ("matmul", (sp[:, 0:128], IDB[:], TRI[:]), dict(start=False, stop=True)),
                                ("matmul", (sp[:, 384:512], KT[ksl, pc, k0 + 128:k0 + 256], QT[ksl, pc, q0 + 128:q0 + 256]), dict(start=True, stop=False)),
                                ("matmul", (sp[:, 384:512], IDB[:], TRI[:]), dict(start=False, stop=True)),
                            ]
                            S.op("pe", insts, reads=rd + [bIDB, bTRI], writes=[spb])
                            S.op("act", [("activation", (), dict(out=pt[:, 0:256], in_=sp[:, 0:256], func=AF.Exp, scale=0.125)),
                                         ("activation", (), dict(out=pt[:, 384:512], in_=sp[:, 384:512], func=AF.Exp, scale=0.125))],
                                 reads=[spb], writes=[ptb])
                        for g in range(2):
                            po, pob = rPO.next()
                            js = [0, 1] if (not own or g == 1) else [0]
                            insts = [("matmul", (po, pt[:, j * 256 + g * 128:j * 256 + g * 128 + 128], VA[:, 2 * n + j, h, 0:65]),
                                      dict(start=(jj == 0), stop=(jj == len(js) - 1))) for jj, j in enumerate(js)]
                            S.op("pe", insts, reads=[ptb] + [bVA[2 * n + j][h // 4] for j in js], writes=[pob])
                            acc, accb = accs[g]
                            ti = 2 * qb + g
                            if n == 0:
                                dve("tensor_scalar", [pob, bMASK[ti]], [accb], out=acc[:], in0=po, scalar1=MASK[:, ti, h, n:n + 1],
                                    scalar2=None, op0=ALU.mult)
                            else:
                                dve("scalar_tensor_tensor", [pob, bMASK[ti], accb], [accb], out=acc[:], in0=po,
                                    scalar=MASK[:, ti, h, n:n + 1], in1=acc[:], op0=ALU.mult, op1=ALU.add)
                    for g in range(2):
                        acc, accb = accs[g]
                        ti = 2 * qb + g
                        sm, smb = rSM.next()
                        dve("reciprocal", [accb], [smb], out=sm[:, 0:1], in_=acc[:, 64:65])
                        dve("tensor_scalar", [accb, smb], [bYA[ti]], out=YA[:, ti, h * 64:(h + 1) * 64], in0=acc[:, 0:64],
                            scalar1=sm[:, 0:1], scalar2=None, op0=ALU.mult)
            for i in range(NT):
                tt = i // 4
                sm, smb = rSM.next()
                sq, sqb = rSQ.next()
                dve("memset", [], [smb], ap=sm[:, 0:1], constant=0.0)
                act(sq[:], YA[:, i, :], AF.Square, [bYA[i], smb], [sqb, smb], accum_out=sm[:, 0:1])
                act(sm[:, 1:2], sm[:, 0:1], AF.Sqrt, [smb, bEPS], [smb], scale=1.0 / 512.0, bias=EPST[:, 0:1])
                dve("reciprocal", [smb], [smb], out=sm[:, 2:3], in_=sm[:, 1:2])
                yn, ynb = rYN.next()
                dve("scalar_tensor_tensor", [bYA[i], smb, bGA], [ynb], out=yn[:], in0=YA[:, i, :], scalar=sm[:, 2:3],
                    in1=GA[:], op0=ALU.mult, op1=ALU.mult)
                ptr, ptrb = rPTR.next()
                S.op("pe", [("transpose", (ptr[:, c * 128:(c + 1) * 128], yn[:, c * 128:(c + 1) * 128], IDB[:]), {}) for c in range(4)],
                     reads=[ynb, bIDB], writes=[ptrb])
                act(YTA[:, :, i * 128:(i + 1) * 128], ptr.rearrange("p (c t) -> p c t", c=4), AF.Copy, [ptrb],
                    [bYTA[c][tt] for c in range(4)])
            S.barrier()
            load_x(s)
            proj_accum(None, lambda kc, tt: (YTA[:, kc, tsl(tt)] if kc < 4 else HT[:, kc - 4, tsl(tt)]),
                       lambda kc, tt: (bYTA[kc][tt] if kc < 4 else bHT[kc - 4][tt]))
            dump(0)
            S.barrier()
            S.dma("sp", MT, mem_d[s], "ldmem", writes=[bMT])
            norm_stage(8, lambda kc, tt: (HT[:, kc, tsl(tt)], bHT[kc][tt]))
            for u in range(4):
                W, wb = consume()
                for ocl in range(2):
                    oc = 2 * u + ocl
                    for tt in range(TT):
                        ps, pb = rPMM.next()
                        mm(ps[:], pb, [(W[:, kc, ocl * 128:(ocl + 1) * 128], HT[:, kc, tsl(tt)]) for kc in range(KC)],
                           [wb] + [bHT[kc][tt] for kc in range(KC)])
                        act(QMT[:, oc, tsl(tt)], ps[:], AF.Copy, [pb], [bQMT[oc][tt]])
            ps, pb = rPMM.next()
            for kc in range(KC):
                sq, sqb = rSQ.next()
                act(sq[:, 0:MEM], MT[:, kc, :], AF.Square, [bMT], [sqb])
                S.op("pe", [("matmul", (ps[:, 0:MEM], ONES[:], sq[:, 0:MEM]), dict(start=(kc == 0), stop=(kc == KC - 1)))],
                     reads=[sqb, bONES], writes=[pb])
            r, rb = rstd_from_ps(ps[:, 0:MEM], pb, MEM, 1.0 / D)
            for kc in range(KC):
                dve("scalar_tensor_tensor", [bMT, rb, bPAR], [bMTB], out=MTB[:, kc, :], in0=MT[:, kc, :],
                    scalar=PAR[:, 16 + kc:17 + kc], in1=r, op0=ALU.mult, op1=ALU.mult)
            for u in range(4):
                W, wb = consume()
                for ocl in range(2):
                    ch = 2 * u + ocl
                    ps, pb = rPMM.next()
                    mm(ps[:, 0:MEM], pb, [(W[:, kc, ocl * 128:(ocl + 1) * 128], MTB[:, kc, :]) for kc in range(KC)], [wb, bMTB])
                    act(MKT[:, ch, :], ps[:, 0:MEM], AF.Copy, [pb], [bMKT])
            for u in range(4):
                W, wb = consume()
                for mt in range(2):
                    ps, pb = rPMM.next()
                    mm(ps[:, 0:256], pb, [(MTB[:, kc, mt * 128:(mt + 1) * 128], W[:, kc, 0:256]) for kc in range(KC)], [wb, bMTB])
                    act(MV[:, mt, u * 256:(u + 1) * 256], ps[:, 0:256], AF.Copy, [pb], [bMV])
            S.barrier()
            for hd in range(4):
                for tt in range(TT):
                    pts = []
                    for kt in range(2):
                        sp, spb = rPSS.next()
                        mm(sp[:], spb, [(MKT[:, 2 * hd + dc, kt * 128:(kt + 1) * 128], QMT[:, 2 * hd + dc, tsl(tt)]) for dc in range(2)],
                           [bMKT, bQMT[2 * hd][tt], bQMT[2 * hd + 1][tt]])
                        pt, ptb = rPT.next()
                        act(pt[:], sp[:], AF.Exp, [spb], [ptb], scale=1.0 / 16.0)
                        pts.append((pt, ptb))
                    pd, pdb = rPMM.next()
                    mm(pd[:], pdb, [(ONES[:], pt[:]) for pt, _ in pts], [bONES] + [b for _, b in pts])
                    rd_, rdb = rTMP.next()
                    dve("reciprocal", [pdb], [rdb], out=rd_[:], in_=pd[:])
                    for dc in range(2):
                        ch = 2 * hd + dc
                        po, pob = rPMM.next()
                        mm(po[:], pob, [(MV[:, kt, ch * 128:(ch + 1) * 128], pts[kt][0][:]) for kt in range(2)],
                           [bMV] + [b for _, b in pts])
                        dve("tensor_tensor", [pob, rdb], [bHT[ch][tt]], out=HT[:, ch, tsl(tt)], in0=po[:], in1=rd_[:], op=ALU.mult)
            proj_accum(None, lambda kc, tt: HT[:, kc, tsl(tt)], lambda kc, tt: bHT[kc][tt])
            dump(1)
            S.barrier()
            for i in range(2):
                dve("memset", [], [rG.items[i][1]], ap=G[i][:, 0:2], constant=0.0)
            norm_stage(24, lambda kc, tt: (HT[:, kc, tsl(tt)], bHT[kc][tt]))
            for (j0, nj) in GROUPS:
                for jl in range(nj):
                    j = j0 + jl
                    W, wb = consume()
                    g_, gb = rG.next()
                    fw = 56 + j * 3
                    for tt in range(TT):
                        pg, pgb = rPMM.next()
                        mm(pg[:], pgb, [(W[:, kc, 0:128], HT[:, kc, tsl(tt)]) for kc in range(KC)], [wb] + [bHT[kc][tt] for kc in range(KC)])
                        pu, pub = rPMM.next()
                        mm(pu[:], pub, [(W[:, kc, 128:256], HT[:, kc, tsl(tt)]) for kc in range(KC)], [wb] + [bHT[kc][tt] for kc in range(KC)])
                        t0 = tt * 512
                        act(g_[:, 2 + t0:2 + t0 + 512], pg[:], AF.Copy, [pgb], [gb])
                        t, tb = rTMP.next()
                        dve("tensor_scalar", [gb, bPAR], [tb], out=t[:], in0=g_[:, t0:t0 + 512], scalar1=PAR[:, fw:fw + 1], scalar2=None, op0=ALU.mult)
                        dve("scalar_tensor_tensor", [gb, tb, bPAR], [tb], out=t[:], in0=g_[:, t0 + 1:t0 + 513],
                            scalar=PAR[:, fw + 1:fw + 2], in1=t[:], op0=ALU.mult, op1=ALU.add)
                        dve("scalar_tensor_tensor", [gb, tb, bPAR], [tb], out=t[:], in0=g_[:, t0 + 2:t0 + 514],
                            scalar=PAR[:, fw + 2:fw + 3], in1=t[:], op0=ALU.mult, op1=ALU.add)
                        sg, sgb = rTMP.next()
                        act(sg[:], t[:], AF.Silu, [tb], [sgb])
                        dve("tensor_tensor", [pub, sgb], [bAT[jl][tt]], out=AT[:, jl, tsl(tt)], in0=pu[:], in1=sg[:], op=ALU.mult)
                for oc in range(KC):
                    W, wb = consume()
                    for tt in range(TT):
                        ps, pb = rPMM.next()
                        mm(ps[:], pb, [(W[:, jl, 0:128], AT[:, jl, tsl(tt)]) for jl in range(nj)], [wb] + [bAT[jl][tt] for jl in range(nj)])
                        dve("tensor_tensor", [pb, bXT[oc][tt]], [bXT[oc][tt]], out=XT[:, oc, tsl(tt)], in0=ps[:],
                            in1=XT[:, oc, tsl(tt)], op=ALU.add)
            dump(2)
            norm_stage(32, lambda kc, tt: (XT[:, kc, tsl(tt)], bXT[kc][tt]))
            for tt in range(TT):
                S.dma("sp", out_d[s, :, :, tsl(tt)], XT[:, :, tsl(tt)], f"ost{tt}", reads=[bXT[k][tt] for k in range(KC)])
            S.barrier()
        assert wstate["next"] == len(units), (wstate, len(units))
        S.final_wait("sp")
        assert S.check_deadlock(), "sync graph deadlocks"
        block = es.enter_context(nc.Block())
        S.emit(block)
    return nc


def _fm(a):
    T, Fd = a.shape[-2], a.shape[-1]
    b = np.swapaxes(a, -1, -2).reshape(a.shape[:-2] + (Fd // 128, 128, T))
    return np.ascontiguousarray(np.swapaxes(b, -3, -2))


def _wl(w):
    K, N = w.shape
    return np.ascontiguousarray(w.reshape(K // 128, 128, N).transpose(1, 0, 2))


def _vec(v):
    return np.ascontiguousarray(v.reshape(-1, 128).T)


def prep_shared(inp):
    f = lambda k: np.asarray(inp[k], dtype=np.float32)
    w_in = f("w_in_mix")[0]
    cols = [w_in[:, 0:1536]]
    for c in range(4):
        for o in (1536, 2048, 2560):
            cols.append(w_in[:, o + c * 128:o + (c + 1) * 128])
    w_in_p = np.concatenate(cols, axis=1)
    w_up = f("w_ffn_up")[0]
    cols = []
    for j in range(NJ):
        cols.append(w_up[:, j * 128:(j + 1) * 128])
        cols.append(w_up[:, DFF + j * 128:DFF + (j + 1) * 128])
    w_up_p = np.concatenate(cols, axis=1)
    par = np.zeros((128, NPAR), np.float32)
    par[:, 0:8] = _vec(f("norm_mix_g")[0])
    par[:, 8:16] = _vec(f("norm_mem_g")[0])
    par[:, 16:24] = _vec(f("mem_kv_norm_g")[0])
    par[:, 24:32] = _vec(f("norm_ffn_g")[0])
    par[:, 32:40] = _vec(f("final_norm_g"))
    par[:, 40:44] = _vec(f("conv_out_g")[0])
    cw = f("conv_mix_w")[0]
    par[:, 44:56] = cw.T.reshape(4, 128, 3).transpose(1, 0, 2).reshape(128, 12)
    fw = f("ffn_conv_w")[0]
    par[:, 56:56 + 66] = fw.T.reshape(NJ, 128, 3).transpose(1, 0, 2).reshape(128, 66)
    cst = np.zeros((128, 320), np.float32)
    cst[:, 0:128] = np.eye(128, dtype=np.float32)
    kk, qq = np.meshgrid(np.arange(128), np.arange(128), indexing="ij")
    cst[:, 128:256] = np.where(kk <= qq, 0.0, NEG).astype(np.float32)
    negm = np.zeros((8, 8), np.float32)
    for qb in range(8):
        for n in range(8):
            negm[qb, n] = 0.0 if n < qb else (1e30 if n == qb else -1e30)
    cst[:, 256:320] = negm.reshape(1, 64)
    return {
        "w_in": _wl(w_in_p), "w_out": _wl(f("w_out_mix")[0]), "w_mq": _wl(f("w_mem_q")[0]),
        "w_mkv": _wl(f("w_mem_kv")[0]), "w_mo": _wl(f("w_mem_o")[0]), "w_up": _wl(w_up_p),
        "w_dn": _wl(f("w_ffn_down")[0]), "par": par,
        "garow": np.ascontiguousarray(f("attn_out_g")[0].reshape(1, 512)), "cst": cst,
    }


_NC_CACHE = {}


def kernel(**inputs):
    n_cores = 8
    x = np.asarray(inputs["x"], dtype=np.float32)
    mem = np.asarray(inputs["mem"], dtype=np.float32)
    Bt = x.shape[0]
    NS = Bt // n_cores
    shared = prep_shared(inputs)
    in_maps = []
    for c in range(n_cores):
        m = dict(shared)
        m["x"] = _fm(x[c * NS:(c + 1) * NS])
        m["mem"] = _fm(mem[c * NS:(c + 1) * NS])
        in_maps.append(m)
    if NS not in _NC_CACHE:
        _NC_CACHE[NS] = build_nc(NS)
    nc = _NC_CACHE[NS]
    res = run_bass_kernel_spmd(nc, in_maps, core_ids=list(range(n_cores)))
    outs = []
    for c in range(n_cores):
        o = res.results[c]["out"]
        o = o.transpose(0, 3, 2, 1).reshape(NS, S_LEN, D)
        outs.append(o)
    return np.ascontiguousarray(np.concatenate(outs, axis=0)).astype(np.float32)
```

```python
import numpy as np
from contextlib import ExitStack
import concourse.bass as bass
import concourse.mybir as mybir
from concourse.bass_utils import run_bass_kernel_spmd

F32 = mybir.dt.float32
BF16 = mybir.dt.bfloat16
AF = mybir.ActivationFunctionType
ALU = mybir.AluOpType
AX = mybir.AxisListType

D = 1024
KC = 8
S_LEN = 2048
TT = 4
NT = 16
MEM = 256
DFF = 2816
NJ = 22
EPS = 1e-6
GROUPS = [(0, 6), (6, 6), (12, 5), (17, 5)]
NPAR = 128
NEG = -30000.0
ATT_DEPTH = 1
CONV_SKEW = 0
FIN_SKEW = 0


class Buf:
    __slots__ = ("name", "w", "r")

    def __init__(self, name):
        self.name = name
        self.w = {}
        self.r = {}


class Sched:
    ENG = ("pe", "act", "dve", "pool", "sp")

    def __init__(self, nc, es):
        self.nc = nc
        self.es = es
        self.streams = {e: [] for e in self.ENG}
        self.cnt = {}
        self.seen = {e: {} for e in self.ENG}
        self.sems = {}
        self.scope = None
        self.use_scopes = False
        for e in ("pe", "act", "dve", "pool"):
            self._sem("E_" + e)

    def _sem(self, key):
        if key not in self.sems:
            self.sems[key] = self.es.enter_context(self.nc.semaphore("s_" + key))
            self.cnt[key] = 0
        return self.sems[key]

    def _deps(self, eng, reads, writes):
        need = {}
        own = "E_" + eng

        def add(d, war=False):
            for k, v in d.items():
                if k == own and eng == "pe":
                    continue
                if need.get(k, 0) < v:
                    need[k] = v
        for b in reads:
            add(b.w)
        for b in writes:
            add(b.w)
            add(b.r, war=True)
        out = []
        seen = self.seen[eng]
        for k, v in need.items():
            if seen.get(k, 0) < v:
                seen[k] = v
                out.append((k, v))
        return out

    def _mark(self, key, val, reads, writes):
        for b in reads:
            if b.r.get(key, 0) < val:
                b.r[key] = val
        for b in writes:
            b.w = {key: val}
            b.r = {}

    def op(self, eng, insts, reads=(), writes=()):
        waits = self._deps(eng, reads, writes)
        key = "E_" + eng
        self.cnt[key] += 1
        val = self.cnt[key]
        self.streams[eng].append((waits, insts, key, 1, self.scope))
        self._mark(key, val, reads, writes)

    def dma(self, q, out_ap, in_ap, semkey, reads=(), writes=()):
        self._sem(semkey)
        waits = self._deps(q, reads, writes)
        self.cnt[semkey] += 16
        val = self.cnt[semkey]
        self.streams[q].append((waits, [("dma_start", (), dict(out=out_ap, in_=in_ap))], semkey, 16, self.scope))
        self._mark(semkey, val, reads, writes)

    def barrier(self, engs=("pe", "act", "dve", "sp")):
        for e in engs:
            waits = []
            for k, v in self.cnt.items():
                if (k == "E_" + e and e == "pe") or v == 0:
                    continue
                if self.seen[e].get(k, 0) < v:
                    self.seen[e][k] = v
                    waits.append((k, v))
            if waits:
                self.streams[e].append((waits, [], None, 0, self.scope))

    def final_wait(self, eng="sp"):
        waits = [(k, v) for k, v in self.cnt.items() if v > 0 and k != "E_" + eng]
        self.streams[eng].append((waits, [], None, 0, self.scope))

    def check_deadlock(self):
        sem = {k: 0 for k in self.cnt}
        pos = {e: 0 for e in self.ENG}
        prog = True
        while prog:
            prog = False
            for e in self.ENG:
                st = self.streams[e]
                while pos[e] < len(st):
                    waits, insts, key, inc, _ = st[pos[e]]
                    if any(sem[k] < v for k, v in waits):
                        break
                    if insts and key is not None:
                        sem[key] += inc
                    pos[e] += 1
                    prog = True
        stuck = {e: (pos[e], len(self.streams[e])) for e in self.ENG if pos[e] < len(self.streams[e])}
        for e in stuck:
            waits, insts, key, inc, _ = self.streams[e][pos[e]]
            print("STUCK", e, pos[e], [(k, v, sem[k]) for k, v in waits if sem[k] < v], [i[0] for i in insts][:2])
        return not stuck

    def emit(self, block):
        hooks = {"pe": block.tensor, "act": block.scalar, "dve": block.vector,
                 "pool": block.gpsimd, "sp": block.sync}
        for eng in self.ENG:
            stream = self.streams[eng]
            if not stream:
                continue

            def body(e, stream=stream):
                cur = None
                cm = None
                for waits, insts, key, inc, scope in stream:
                    if self.use_scopes and scope != cur:
                        if cm is not None:
                            cm.__exit__(None, None, None)
                        cm = self.nc.named_scope(scope) if scope else None
                        if cm is not None:
                            cm.__enter__()
                        cur = scope
                    for k, v in waits:
                        e.wait_ge(self.sems[k], v)
                    ins = None
                    for m, a, kw in insts:
                        ins = getattr(e, m)(*a, **kw)
                    if ins is not None and key is not None:
                        ins.then_inc(self.sems[key], inc)
                if cm is not None:
                    cm.__exit__(None, None, None)
            hooks[eng](body)


class Rot:
    ALL = []

    def __init__(self, items):
        self.items = items
        self.i = 0
        Rot.ALL.append(self)

    def next(self):
        x = self.items[self.i % len(self.items)]
        self.i += 1
        return x


def build_nc(NS, debug=False, scopes=False):
    Rot.ALL = []
    nc = bass.Bass("TRN2", target_bir_lowering=False)
    dt = lambda n, s, k="ExternalInput": nc.dram_tensor(n, s, F32, kind=k).ap()
    x_d = dt("x", [NS, 128, KC, S_LEN])
    mem_d = dt("mem", [NS, 128, KC, MEM])
    win_d = dt("w_in", [128, KC, 3072])
    wout_d = dt("w_out", [128, KC, 1024])
    wmq_d = dt("w_mq", [128, KC, 1024])
    wmkv_d = dt("w_mkv", [128, KC, 2048])
    wmo_d = dt("w_mo", [128, KC, 1024])
    wup_d = dt("w_up", [128, KC, 2 * DFF])
    wdn_d = dt("w_dn", [128, NJ, 1024])
    par_d = dt("par", [128, NPAR])
    garow_d = dt("garow", [1, 512])
    cst_d = dt("cst", [128, 320])
    out_d = dt("out", [NS, 128, KC, S_LEN], "ExternalOutput")
    if debug:
        dbg_d = dt("dbg", [3, 128, KC, S_LEN], "ExternalOutput")

    es = ExitStack()
    with es:
        S = Sched(nc, es)
        S.use_scopes = scopes
        sb = lambda n, s, d=F32: es.enter_context(nc.sbuf_tensor(n, s, d))
        XT = sb("XT", [128, KC, S_LEN])
        HT = sb("HT", [128, KC, S_LEN], BF16)
        SCR = sb("SCR", [128, 10248])
        scrb = SCR[:, :].bitcast(BF16)
        YTA = scrb[:, 0:8192].rearrange("p (a b) -> p a b", a=4)
        RING = sb("RING", [128, 6, 2048], BF16)
        PAR = sb("PAR", [128, NPAR])
        GA = sb("GA", [128, 512])
        CST = sb("CST", [128, 320])
        IDB = sb("IDB", [128, 128], BF16)
        TRI = sb("TRI", [128, 128], BF16)
        ONES = sb("ONES", [128, 128], BF16)
        EPST = sb("EPST", [128, 1])
        SQ = [sb(f"SQ{i}", [128, 512], BF16) for i in range(3)]
        RT = [sb(f"RT{i}", [128, 512]) for i in range(2)]
        TMP = [sb(f"TMP{i}", [128, 512]) for i in range(4)]
        YC = [SCR[:, 4096 + i * 512:4096 + (i + 1) * 512] for i in range(4)]
        CU = [SCR[:, 6144 + i * 514:6144 + (i + 1) * 514] for i in range(4)]
        PT = [sb(f"PT{i}", [128, 512], BF16) for i in range(4)]
        ACC = [sb(f"ACC{i}", [128, 65]) for i in range(4)]
        SM = [sb(f"SM{i}", [128, 4]) for i in range(4)]
        GS = [sb(f"GS{i}", [128, 8, 8]) for i in range(2)]
        TOP = [sb(f"TOP{i}", [128, 8, 8]) for i in range(2)]
        KMS = sb("KMS", [128, 4, 8])
        KMB = sb("KMB", [128, 4, 8], BF16)
        YN = [sb(f"YN{i}", [128, 512], BF16) for i in range(2)]
        MT = SCR[:, 8192:10240].rearrange("p (a b) -> p a b", a=KC)
        MTB = sb("MTB", [128, KC, MEM], BF16)
        MKT = sb("MKT", [128, KC, MEM], BF16)
        MV = sb("MV", [128, 2, 1024], BF16)
        G = [SCR[:, 6144 + i * 2050:6144 + (i + 1) * 2050] for i in range(2)]
        AT = scrb[:, 0:12288].rearrange("p (a b) -> p a b", a=6)
        xflat = XT[:, :, :].rearrange("p a b -> p (a b)")
        xbf = xflat.bitcast(BF16) if hasattr(xflat, "bitcast") else None
        assert xbf is not None
        KT = xbf[:, 0:8192].rearrange("p (a b) -> p a b", a=4)
        QT = xbf[:, 8192:16384].rearrange("p (a b) -> p a b", a=4)
        VAF = xbf[:, 16384:16384 + 16 * 8 * 66]
        VA = xbf[:, 16384:16384 + 16 * 8 * 66].rearrange("p (a b c) -> p a b c", a=16, b=8)
        mo = (16384 + 16 * 8 * 66) // 2
        MASK = xflat[:, mo:mo + 1024].rearrange("p (a b c) -> p a b c", a=16, b=8)
        QMT = scrb[:, 0:16384].rearrange("p (a b) -> p a b", a=KC)
        pst = lambda n, s, d=F32: es.enter_context(nc.psum_tensor(n, s, d))
        PB = [pst(f"PB{i}", [128, 512]) for i in range(8)]
        bPB = [Buf(f"PB{i}") for i in range(8)]
        B = lambda n: Buf(n)
        bXT = [[B(f"XT{o}_{t}") for t in range(TT)] for o in range(KC)]
        bHT = [[B(f"HT{o}_{t}") for t in range(TT)] for o in range(KC)]
        bYTA = [[B(f"YTA{o}_{t}") for t in range(TT)] for o in range(4)]
        bRING = [B(f"RING{i}") for i in range(6)]
        bPAR, bGA, bCST, bIDB, bTRI, bONES, bEPS = B("PAR"), B("GA"), B("CST"), B("IDB"), B("TRI"), B("ONES"), B("EPS")
        rSQ = Rot([(SQ[i], B(f"SQ{i}")) for i in range(3)])
        rRT = Rot([(RT[i], B(f"RT{i}")) for i in range(2)])
        rTMP = Rot([(TMP[i], B(f"TMP{i}")) for i in range(4)])
        bYC = [B(f"YC{i}") for i in range(4)]
        bCU = [B(f"CU{i}") for i in range(4)]
        rPT = Rot([(PT[i], B(f"PT{i}")) for i in range(4)])
        rCT = Rot([(SCR[:, i * 512:(i + 1) * 512], B(f"CT{i}")) for i in range(8)])
        rACC = Rot([(ACC[i], B(f"ACC{i}")) for i in range(4)])
        rSM = Rot([(SM[i], B(f"SM{i}")) for i in range(4)])
        rGS = Rot([(GS[i], TOP[i], B(f"GS{i}"), B(f"TOP{i}")) for i in range(2)])
        bKMS, bKMB = B("KMS"), B("KMB")
        rYN = Rot([(YN[i], B(f"YN{i}")) for i in range(2)])
        bMT, bMTB, bMKT, bMV = B("MT"), B("MTB"), B("MKT"), B("MV")
        rG = Rot([(G[i], B(f"G{i}")) for i in range(2)])
        bAT = [[B(f"AT{j}_{t}") for t in range(TT)] for j in range(6)]
        bKT = [[B(f"KT{p}_{t}") for t in range(TT)] for p in range(4)]
        bQT = [[B(f"QT{p}_{t}") for t in range(TT)] for p in range(4)]
        bVA = [[B(f"VA{i}_{u}") for u in range(2)] for i in range(NT)]
        bMASK = [B(f"MASK{i}") for i in range(NT)]
        bQMT = [[B(f"QMT{o}_{t}") for t in range(TT)] for o in range(KC)]
        rPMM = Rot([(PB[i], bPB[i]) for i in range(4)])
        rPTR = Rot([(PB[i][:, :].bitcast(BF16)[:, 0:512], bPB[i]) for i in (6, 7)])
        rPSS = Rot([(PB[i], bPB[i]) for i in (4, 5)])
        rPO = Rot([(PB[i][:, 0:65], bPB[i]) for i in (0, 1, 2, 3, 6, 7)])
        rPSA = Rot([(PB[i], bPB[i]) for i in (4, 5)])
        rPOA = Rot([(PB[i][:, 0:65], bPB[i]) for i in (0, 1, 2, 3, 6, 7)])

        tsl = lambda t: slice(t * 512, (t + 1) * 512)

        units = []

        def add_units(w_d, c0, ncols, rows=KC, r0=0, step=256):
            for c in range(c0, c0 + ncols, step):
                units.append((w_d[:, r0:r0 + rows, c:c + step], rows, step))
        for s in range(NS):
            add_units(win_d, 512, 512)
            add_units(win_d, 1024, 512)
            add_units(win_d, 0, 512)
            add_units(win_d, 1536, 1536)
            add_units(wout_d, 0, 1024)
            add_units(wmq_d, 0, 1024)
            add_units(wmkv_d, 0, 2048)
            add_units(wmo_d, 0, 1024)
            for (j0, nj) in GROUPS:
                add_units(wup_d, j0 * 256, nj * 256)
                add_units(wdn_d, 0, 1024, rows=nj, r0=j0, step=128)
        wstate = {"issued": 0, "next": 0}
        LOOK = 5

        def issue_loads(upto):
            while wstate["issued"] < min(upto, len(units)):
                i = wstate["issued"]
                ap, rows, cols = units[i]
                sl = i % 6
                dst = RING[:, sl, 0:rows * cols].rearrange("p (r c) -> p r c", r=rows)
                S.dma("pool", dst, ap, f"ring{sl}", writes=[bRING[sl]])
                wstate["issued"] += 1

        def consume(cap=None):
            i = wstate["next"]
            wstate["next"] += 1
            issue_loads(i + LOOK + 1 if cap is None else min(i + LOOK + 1, cap))
            ap, rows, cols = units[i]
            sl = i % 6
            return RING[:, sl, 0:rows * cols].rearrange("p (r c) -> p r c", r=rows), bRING[sl]

        def mm(out_ap, out_b, pairs, reads):
            n = len(pairs)
            insts = [("matmul", (out_ap, l, r), dict(start=(i == 0), stop=(i == n - 1)))
                     for i, (l, r) in enumerate(pairs)]
            S.op("pe", insts, reads=reads, writes=[out_b])

        def act(out_ap, in_ap, func, reads, writes, **kw):
            S.op("act", [("activation", (), dict(out=out_ap, in_=in_ap, func=func, **kw))], reads=reads, writes=writes)

        def dve(method, reads, writes, **kw):
            S.op("dve", [(method, (), kw)], reads=reads, writes=writes)

        def rstd_from_ps(ps_ap, ps_b, n, inv_d):
            rt, rb = rRT.next()
            act(rt[:, 0:n], ps_ap, AF.Sqrt, [ps_b, bEPS], [rb], scale=inv_d, bias=EPST[:, 0:1])
            dve("reciprocal", [rb], [rb], out=rt[:, 0:n], in_=rt[:, 0:n])
            return rt[:, 0:n], rb

        def norm_stage(gcol, dst_fn):
            for tt in range(TT):
                ps, pb = rPMM.next()
                for kc in range(KC):
                    sq, sqb = rSQ.next()
                    act(sq[:], XT[:, kc, tsl(tt)], AF.Square, [bXT[kc][tt]], [sqb])
                    S.op("pe", [("matmul", (ps[:], ONES[:], sq[:]), dict(start=(kc == 0), stop=(kc == KC - 1)))],
                         reads=[sqb, bONES], writes=[pb])
                r, rb = rstd_from_ps(ps[:], pb, 512, 1.0 / D)
                for kc in range(KC):
                    o_ap, o_b = dst_fn(kc, tt)
                    dve("scalar_tensor_tensor", [bXT[kc][tt], rb, bPAR], [o_b], out=o_ap, in0=XT[:, kc, tsl(tt)],
                        scalar=PAR[:, gcol + kc:gcol + kc + 1], in1=r, op0=ALU.mult, op1=ALU.mult)

        def load_x(s):
            for tt in range(TT):
                S.dma("sp", XT[:, :, tsl(tt)], x_d[s, :, :, tsl(tt)], f"xld{tt}", writes=[bXT[k][tt] for k in range(KC)])

        def proj_accum(w_d_units, src, src_b, dst_add=True):
            for u in range(4):
                W, wb = consume()
                for ocl in range(2):
                    oc = 2 * u + ocl
                    for tt in range(TT):
                        ps, pb = rPMM.next()
                        mm(ps[:], pb, [(W[:, kc, ocl * 128:(ocl + 1) * 128], src(kc, tt)) for kc in range(KC)],
                           [wb] + [src_b(kc, tt) for kc in range(KC)])
                        dve("tensor_tensor", [pb, bXT[oc][tt]], [bXT[oc][tt]], out=XT[:, oc, tsl(tt)], in0=ps[:],
                            in1=XT[:, oc, tsl(tt)], op=ALU.add)

        def dump(idx):
            if not debug:
                return
            S.barrier(("sp",))
            for tt in range(TT):
                S.dma("sp", dbg_d[idx, :, :, tsl(tt)], XT[:, :, tsl(tt)], f"dbg{tt}", reads=[bXT[k][tt] for k in range(KC)])

        S.dma("sp", PAR[:], par_d, "ldpar", writes=[bPAR])
        S.dma("sp", GA[:], garow_d.to_broadcast([128, 512]), "ldga", writes=[bGA])
        S.dma("sp", CST[:], cst_d, "ldcst", writes=[bCST])
        dve("tensor_copy", [bCST], [bIDB], out=IDB[:], in_=CST[:, 0:128])
        dve("tensor_copy", [bCST], [bTRI], out=TRI[:], in_=CST[:, 128:256])
        dve("memset", [], [bONES], ap=ONES[:], constant=1.0)
        dve("memset", [], [bEPS], ap=EPST[:], constant=EPS)
        NEGM = CST[:, 256:320].rearrange("p (a b) -> p a b", a=8)

        for s in range(NS):
            for r_ in Rot.ALL:
                r_.i = 0
            S.scope = "A_norm"
            load_x(s)
            norm_stage(0, lambda kc, tt: (HT[:, kc, tsl(tt)], bHT[kc][tt]))
            S.barrier()
            dve("memset", [], [b for i in range(NT) for b in bVA[i]], ap=VAF, constant=1.0)
            for c in range(4):
                dve("memset", [], [bCU[c]], ap=CU[c][:, 0:2], constant=0.0)
            S.scope = "A_kvq"
            for u in range(2):
                W, wb = consume()
                for pcl in range(2):
                    pc = 2 * u + pcl
                    for tt in range(TT):
                        ps, pb = rPMM.next()
                        mm(ps[:], pb, [(W[:, kc, pcl * 128:(pcl + 1) * 128], HT[:, kc, tsl(tt)]) for kc in range(KC)],
                           [wb] + [bHT[kc][tt] for kc in range(KC)])
                        act(KT[:, pc, tsl(tt)], ps[:], AF.Copy, [pb], [bKT[pc][tt]])
            for u in range(2):
                W, wb = consume()
                for i in range(NT):
                    ps, pb = rPMM.next()
                    tt, o = i // 4, (i % 4) * 128
                    mm(ps[:, 0:256], pb, [(HT[:, kc, tt * 512 + o:tt * 512 + o + 128], W[:, kc, 0:256]) for kc in range(KC)],
                       [wb] + [bHT[kc][tt] for kc in range(KC)])
                    dve("tensor_copy", [pb], [bVA[i][u]], out=VA[:, i, 4 * u:4 * u + 4, 0:64],
                        in_=ps[:, 0:256].rearrange("p (h d) -> p h d", h=4))
            for u in range(2):
                W, wb = consume()
                for pcl in range(2):
                    pc = 2 * u + pcl
                    for tt in range(TT):
                        ps, pb = rPMM.next()
                        mm(ps[:], pb, [(W[:, kc, pcl * 128:(pcl + 1) * 128], HT[:, kc, tsl(tt)]) for kc in range(KC)],
                           [wb] + [bHT[kc][tt] for kc in range(KC)])
                        act(QT[:, pc, tsl(tt)], ps[:], AF.Copy, [pb], [bQT[pc][tt]])
            S.scope = "A_gate"
            dve("tensor_reduce", [bKT[p][t] for p in range(4) for t in range(TT)], [bKMS],
                out=KMS[:, :, :].rearrange("p a b -> p (a b)"),
                in_=KT.rearrange("p a (n l) -> p (a n) l", l=256), axis=AX.X, op=ALU.add)
            act(KMB[:], KMS[:], AF.Copy, [bKMS], [bKMB], scale=1.0 / 256.0)
            for i in range(NT):
                qb = i // 2
                tt = i // 4
                ps, pb = rPMM.next()
                insts = []
                for h in range(8):
                    pc, hp = h // 2, h % 2
                    insts.append(("matmul", (ps[:, h * 8:h * 8 + 8], QT[hp * 64:hp * 64 + 64, pc, i * 128:(i + 1) * 128],
                                             KMB[hp * 64:hp * 64 + 64, pc, :]), dict(start=True, stop=True)))
                S.op("pe", insts, reads=[bKMB] + [bQT[p][tt] for p in range(4)], writes=[pb])
                gs, top, gsb, topb = rGS.next()
                dve("tensor_tensor", [pb, bCST], [gsb], out=gs[:], in0=ps[:, 0:64].rearrange("p (h n) -> p h n", h=8),
                    in1=NEGM[:, qb:qb + 1, :].to_broadcast([128, 8, 8]), op=ALU.add)
                S.op("dve", [("max", (), dict(out=top[:, h, :], in_=gs[:, h, :])) for h in range(8)], reads=[gsb], writes=[topb])
                dve("tensor_tensor", [gsb, topb], [bMASK[i]], out=MASK[:, i, :, :], in0=gs[:],
                    in1=top[:, :, 3:4].to_broadcast([128, 8, 8]), op=ALU.is_ge)
            S.scope = "A_conv"
            cap = wstate["next"] + 6
            Wc = [consume(cap) for _ in range(6)]

            def wblk(c, which):
                blk = c * 3 + which
                W, wb = Wc[blk // 2]
                return W, wb, (blk % 2) * 128
            for tt in range(TT):
                pss, pssb = rPSS.next()
                for c in range(4):
                    pp = []
                    for which in range(3):
                        W, wb, co = wblk(c, which)
                        ps, pb = rPMM.next()
                        mm(ps[:], pb, [(W[:, kc, co:co + 128], HT[:, kc, tsl(tt)]) for kc in range(KC)],
                           [wb] + [bHT[kc][tt] for kc in range(KC)])
                        pp.append((ps, pb))
                        if which == 1:
                            cs, csb = rTMP.next()
                            act(cs[:], ps[:], AF.Copy, [pb], [csb])
                    (pB, pBb), (pC, pCb), (pU, pUb) = pp
                    dve("tensor_tensor", [pUb, csb], [bCU[c]], out=CU[c][:, 2:514], in0=pU[:], in1=cs[:], op=ALU.mult)
                    t, tb = rTMP.next()
                    cw = 44 + c * 3
                    act(t[:], CU[c][:, 2:514], AF.Copy, [bCU[c], bPAR], [tb], scale=PAR[:, cw + 2:cw + 3])
                    dve("scalar_tensor_tensor", [bCU[c], tb, bPAR], [tb], out=t[:], in0=CU[c][:, 1:513],
                        scalar=PAR[:, cw + 1:cw + 2], in1=t[:], op0=ALU.mult, op1=ALU.add)
                    dve("scalar_tensor_tensor", [bCU[c], tb, bPAR], [tb], out=t[:], in0=CU[c][:, 0:512],
                        scalar=PAR[:, cw:cw + 1], in1=t[:], op0=ALU.mult, op1=ALU.add)
                    dve("tensor_tensor", [pBb, tb], [bYC[c]], out=YC[c][:], in0=pB[:], in1=t[:], op=ALU.mult)
                    dve("tensor_copy", [bCU[c]], [bCU[c]], out=CU[c][:, 0:2], in_=CU[c][:, 512:514])
                    sq, sqb = rSQ.next()
                    act(sq[:], YC[c][:], AF.Square, [bYC[c]], [sqb])
                    S.op("pe", [("matmul", (pss[:], ONES[:], sq[:]), dict(start=(c == 0), stop=(c == 3)))],
                         reads=[sqb, bONES], writes=[pssb])
                r, rb = rstd_from_ps(pss[:], pssb, 512, 1.0 / 512.0)
                for c in range(4):
                    dve("scalar_tensor_tensor", [bYC[c], rb, bPAR], [bHT[c][tt]], out=HT[:, c, tsl(tt)], in0=YC[c][:],
                        scalar=PAR[:, 40 + c:41 + c], in1=r, op0=ALU.mult, op1=ALU.mult)
            pass
            S.barrier()
            S.scope = "A_attn"
            YA = HT[:, 4:8, :].rearrange("p a (t f) -> p (a t) f", f=512)
            bYA = [B(f"YA{s}_{i}") for i in range(NT)]
            steps = [(h, qb, n) for h in range(8) for qb in range(8) for n in range(qb + 1)]
            st_state = {}
            acc_of = {}

            def attn_front(i):
                h, qb, n = steps[i]
                pc, hp = h // 2, h % 2
                ksl = slice(hp * 64, hp * 64 + 64)
                q0 = qb * 256
                tq = qb // 2
                own = (n == qb)
                tk = n // 2
                sp, spb = rPSA.next()
                pt, ptb = rPT.next()
                rd = [bKT[pc][tk], bQT[pc][tq]]
                if not own:
                    insts = [("matmul", (sp[:, j * 256:(j + 1) * 256], KT[ksl, pc, (2 * n + j) * 128:(2 * n + j + 1) * 128],
                                         QT[ksl, pc, q0:q0 + 256]), dict(start=True, stop=True)) for j in range(2)]
                    S.op("pe", insts, reads=rd, writes=[spb])
                    act(pt[:], sp[:], AF.Exp, [spb], [ptb], scale=0.125)
                else:
                    k0 = 2 * n * 128
                    insts = [
                        ("matmul", (sp[:, 128:256], KT[ksl, pc, k0:k0 + 128], QT[ksl, pc, q0 + 128:q0 + 256]), dict(start=True, stop=True)),
                        ("matmul", (sp[:, 0:128], KT[ksl, pc, k0:k0 + 128], QT[ksl, pc, q0:q0 + 128]), dict(start=True, stop=False)),
                        ("matmul", (sp[:, 0:128], IDB[:], TRI[:]), dict(start=False, stop=True)),
                        ("matmul", (sp[:, 384:512], KT[ksl, pc, k0 + 128:k0 + 256], QT[ksl, pc, q0 + 128:q0 + 256]), dict(start=True, stop=False)),
                        ("matmul", (sp[:, 384:512], IDB[:], TRI[:]), dict(start=False, stop=True)),
                    ]
                    S.op("pe", insts, reads=rd + [bIDB, bTRI], writes=[spb])
                    S.op("act", [("activation", (), dict(out=pt[:, 0:256], in_=sp[:, 0:256], func=AF.Exp, scale=0.125)),
                                 ("activation", (), dict(out=pt[:, 384:512], in_=sp[:, 384:512], func=AF.Exp, scale=0.125))],
                         reads=[spb], writes=[ptb])
                st_state[i] = (pt, ptb)

            def attn_back(i):
                h, qb, n = steps[i]
                own = (n == qb)
                pt, ptb = st_state.pop(i)
                if n == 0:
                    acc_of[(h, qb)] = [rACC.next() for _ in range(2)]
                accs = acc_of[(h, qb)]
                for g in range(2):
                    po, pob = rPOA.next()
                    js = [0, 1] if (not own or g == 1) else [0]
                    insts = [("matmul", (po, pt[:, j * 256 + g * 128:j * 256 + g * 128 + 128], VA[:, 2 * n + j, h, 0:65]),
                              dict(start=(jj == 0), stop=(jj == len(js) - 1))) for jj, j in enumerate(js)]
                    S.op("pe", insts, reads=[ptb] + [bVA[2 * n + j][h // 4] for j in js], writes=[pob])
                    acc, accb = accs[g]
                    ti = 2 * qb + g
                    if n == 0:
                        dve("tensor_scalar", [pob, bMASK[ti]], [accb], out=acc[:], in0=po, scalar1=MASK[:, ti, h, n:n + 1],
                            scalar2=None, op0=ALU.mult)
                    else:
                        dve("scalar_tensor_tensor", [pob, bMASK[ti], accb], [accb], out=acc[:], in0=po,
                            scalar=MASK[:, ti, h, n:n + 1], in1=acc[:], op0=ALU.mult, op1=ALU.add)
                if own:
                    for g in range(2):
                        acc, accb = accs[g]
                        ti = 2 * qb + g
                        sm, smb = rSM.next()
                        dve("reciprocal", [accb], [smb], out=sm[:, 0:1], in_=acc[:, 64:65])
                        dve("tensor_scalar", [accb, smb], [bYA[ti]], out=YA[:, ti, h * 64:(h + 1) * 64], in0=acc[:, 0:64],
                            scalar1=sm[:, 0:1], scalar2=None, op0=ALU.mult)
                    del acc_of[(h, qb)]
            DEPTH = ATT_DEPTH
            for i in range(len(steps) + DEPTH):
                if i < len(steps):
                    attn_front(i)
                if i - DEPTH >= 0:
                    attn_back(i - DEPTH)
            S.scope = "A_fin"
            fin_yn = {}

            def fin_front(i):
                sm, smb = rSM.next()
                sq, sqb = rSQ.next()
                dve("memset", [], [smb], ap=sm[:, 0:1], constant=0.0)
                act(sq[:], YA[:, i, :], AF.Square, [bYA[i], smb], [sqb, smb], accum_out=sm[:, 0:1])
                act(sm[:, 1:2], sm[:, 0:1], AF.Sqrt, [smb, bEPS], [smb], scale=1.0 / 512.0, bias=EPST[:, 0:1])
                dve("reciprocal", [smb], [smb], out=sm[:, 2:3], in_=sm[:, 1:2])
                yn, ynb = rYN.next()
                dve("scalar_tensor_tensor", [bYA[i], smb, bGA], [ynb], out=yn[:], in0=YA[:, i, :], scalar=sm[:, 2:3],
                    in1=GA[:], op0=ALU.mult, op1=ALU.mult)
                fin_yn[i] = (yn, ynb)

            def fin_back(i):
                tt = i // 4
                yn, ynb = fin_yn.pop(i)
                ptr, ptrb = rPTR.next()
                S.op("pe", [("transpose", (ptr[:, c * 128:(c + 1) * 128], yn[:, c * 128:(c + 1) * 128], IDB[:]), {}) for c in range(4)],
                     reads=[ynb, bIDB], writes=[ptrb])
                act(YTA[:, :, i * 128:(i + 1) * 128], ptr.rearrange("p (c t) -> p c t", c=4), AF.Copy, [ptrb],
                    [bYTA[c][tt] for c in range(4)])
            for i in range(NT + FIN_SKEW):
                if i < NT:
                    fin_front(i)
                if i >= FIN_SKEW:
                    fin_back(i - FIN_SKEW)
            S.barrier()
            S.scope = "A_out"
            load_x(s)
            proj_accum(None, lambda kc, tt: (YTA[:, kc, tsl(tt)] if kc < 4 else HT[:, kc - 4, tsl(tt)]),
                       lambda kc, tt: (bYTA[kc][tt] if kc < 4 else bHT[kc - 4][tt]))
            dump(0)
            S.barrier()
            S.scope = "B_proj"
            S.dma("sp", MT, mem_d[s], "ldmem", writes=[bMT])
            norm_stage(8, lambda kc, tt: (HT[:, kc, tsl(tt)], bHT[kc][tt]))
            for u in range(4):
                W, wb = consume()
                for ocl in range(2):
                    oc = 2 * u + ocl
                    for tt in range(TT):
                        ps, pb = rPMM.next()
                        mm(ps[:], pb, [(W[:, kc, ocl * 128:(ocl + 1) * 128], HT[:, kc, tsl(tt)]) for kc in range(KC)],
                           [wb] + [bHT[kc][tt] for kc in range(KC)])
                        act(QMT[:, oc, tsl(tt)], ps[:], AF.Copy, [pb], [bQMT[oc][tt]])
            ps, pb = rPMM.next()
            for kc in range(KC):
                sq, sqb = rSQ.next()
                act(sq[:, 0:MEM], MT[:, kc, :], AF.Square, [bMT], [sqb])
                S.op("pe", [("matmul", (ps[:, 0:MEM], ONES[:], sq[:, 0:MEM]), dict(start=(kc == 0), stop=(kc == KC - 1)))],
                     reads=[sqb, bONES], writes=[pb])
            r, rb = rstd_from_ps(ps[:, 0:MEM], pb, MEM, 1.0 / D)
            for kc in range(KC):
                dve("scalar_tensor_tensor", [bMT, rb, bPAR], [bMTB], out=MTB[:, kc, :], in0=MT[:, kc, :],
                    scalar=PAR[:, 16 + kc:17 + kc], in1=r, op0=ALU.mult, op1=ALU.mult)
            for u in range(4):
                W, wb = consume()
                for ocl in range(2):
                    ch = 2 * u + ocl
                    ps, pb = rPMM.next()
                    mm(ps[:, 0:MEM], pb, [(W[:, kc, ocl * 128:(ocl + 1) * 128], MTB[:, kc, :]) for kc in range(KC)], [wb, bMTB])
                    act(MKT[:, ch, :], ps[:, 0:MEM], AF.Copy, [pb], [bMKT])
            for u in range(4):
                W, wb = consume()
                for mt in range(2):
                    ps, pb = rPMM.next()
                    mm(ps[:, 0:256], pb, [(MTB[:, kc, mt * 128:(mt + 1) * 128], W[:, kc, 0:256]) for kc in range(KC)], [wb, bMTB])
                    act(MV[:, mt, u * 256:(u + 1) * 256], ps[:, 0:256], AF.Copy, [pb], [bMV])
            S.barrier()
            S.scope = "B_attn"
            for hd in range(4):
                for tt in range(TT):
                    pts = []
                    for kt in range(2):
                        sp, spb = rPSS.next()
                        mm(sp[:], spb, [(MKT[:, 2 * hd + dc, kt * 128:(kt + 1) * 128], QMT[:, 2 * hd + dc, tsl(tt)]) for dc in range(2)],
                           [bMKT, bQMT[2 * hd][tt], bQMT[2 * hd + 1][tt]])
                        pt, ptb = rPT.next()
                        act(pt[:], sp[:], AF.Exp, [spb], [ptb], scale=1.0 / 16.0)
                        pts.append((pt, ptb))
                    pd, pdb = rPMM.next()
                    mm(pd[:], pdb, [(ONES[:], pt[:]) for pt, _ in pts], [bONES] + [b for _, b in pts])
                    rd_, rdb = rTMP.next()
                    dve("reciprocal", [pdb], [rdb], out=rd_[:], in_=pd[:])
                    for dc in range(2):
                        ch = 2 * hd + dc
                        po, pob = rPMM.next()
                        mm(po[:], pob, [(MV[:, kt, ch * 128:(ch + 1) * 128], pts[kt][0][:]) for kt in range(2)],
                           [bMV] + [b for _, b in pts])
                        dve("tensor_tensor", [pob, rdb], [bHT[ch][tt]], out=HT[:, ch, tsl(tt)], in0=po[:], in1=rd_[:], op=ALU.mult)
            proj_accum(None, lambda kc, tt: HT[:, kc, tsl(tt)], lambda kc, tt: bHT[kc][tt])
            dump(1)
            S.barrier()
            S.scope = "C_ffn"
            for i in range(2):
                dve("memset", [], [rG.items[i][1]], ap=G[i][:, 0:2], constant=0.0)
            norm_stage(24, lambda kc, tt: (HT[:, kc, tsl(tt)], bHT[kc][tt]))
            for (j0, nj) in GROUPS:
                for jl in range(nj):
                    j = j0 + jl
                    W, wb = consume()
                    g_, gb = rG.next()
                    fw = 56 + j * 3
                    for tt in range(TT):
                        pg, pgb = rPMM.next()
                        mm(pg[:], pgb, [(W[:, kc, 0:128], HT[:, kc, tsl(tt)]) for kc in range(KC)], [wb] + [bHT[kc][tt] for kc in range(KC)])
                        pu, pub = rPMM.next()
                        mm(pu[:], pub, [(W[:, kc, 128:256], HT[:, kc, tsl(tt)]) for kc in range(KC)], [wb] + [bHT[kc][tt] for kc in range(KC)])
                        t0 = tt * 512
                        act(g_[:, 2 + t0:2 + t0 + 512], pg[:], AF.Copy, [pgb], [gb])
                        t, tb = rTMP.next()
                        dve("tensor_scalar", [gb, bPAR], [tb], out=t[:], in0=g_[:, t0:t0 + 512], scalar1=PAR[:, fw:fw + 1], scalar2=None, op0=ALU.mult)
                        dve("scalar_tensor_tensor", [gb, tb, bPAR], [tb], out=t[:], in0=g_[:, t0 + 1:t0 + 513],
                            scalar=PAR[:, fw + 1:fw + 2], in1=t[:], op0=ALU.mult, op1=ALU.add)
                        dve("scalar_tensor_tensor", [gb, tb, bPAR], [tb], out=t[:], in0=g_[:, t0 + 2:t0 + 514],
                            scalar=PAR[:, fw + 2:fw + 3], in1=t[:], op0=ALU.mult, op1=ALU.add)
                        sg, sgb = rTMP.next()
                        act(sg[:], t[:], AF.Silu, [tb], [sgb])
                        dve("tensor_tensor", [pub, sgb], [bAT[jl][tt]], out=AT[:, jl, tsl(tt)], in0=pu[:], in1=sg[:], op=ALU.mult)
                for oc in range(KC):
                    W, wb = consume()
                    for tt in range(TT):
                        ps, pb = rPMM.next()
                        mm(ps[:], pb, [(W[:, jl, 0:128], AT[:, jl, tsl(tt)]) for jl in range(nj)], [wb] + [bAT[jl][tt] for jl in range(nj)])
                        dve("tensor_tensor", [pb, bXT[oc][tt]], [bXT[oc][tt]], out=XT[:, oc, tsl(tt)], in0=ps[:],
                            in1=XT[:, oc, tsl(tt)], op=ALU.add)
            dump(2)
            S.scope = "C_final"
            norm_stage(32, lambda kc, tt: (XT[:, kc, tsl(tt)], bXT[kc][tt]))
            for tt in range(TT):
                S.dma("sp", out_d[s, :, :, tsl(tt)], XT[:, :, tsl(tt)], f"ost{tt}", reads=[bXT[k][tt] for k in range(KC)])
            S.barrier()
        assert wstate["next"] == len(units), (wstate, len(units))
        S.final_wait("sp")
        assert S.check_deadlock(), "sync graph deadlocks"
        block = es.enter_context(nc.Block())
        S.emit(block)
    return nc


def _fm(a):
    T, Fd = a.shape[-2], a.shape[-1]
    b = np.swapaxes(a, -1, -2).reshape(a.shape[:-2] + (Fd // 128, 128, T))
    return np.ascontiguousarray(np.swapaxes(b, -3, -2))


def _wl(w):
    K, N = w.shape
    return np.ascontiguousarray(w.reshape(K // 128, 128, N).transpose(1, 0, 2))


def _vec(v):
    return np.ascontiguousarray(v.reshape(-1, 128).T)


def prep_shared(inp):
    f = lambda k: np.asarray(inp[k], dtype=np.float32)
    w_in = f("w_in_mix")[0]
    cols = [w_in[:, 0:1536]]
    for c in range(4):
        for o in (1536, 2048, 2560):
            cols.append(w_in[:, o + c * 128:o + (c + 1) * 128])
    w_in_p = np.concatenate(cols, axis=1)
    w_up = f("w_ffn_up")[0]
    cols = []
    for j in range(NJ):
        cols.append(w_up[:, j * 128:(j + 1) * 128])
        cols.append(w_up[:, DFF + j * 128:DFF + (j + 1) * 128])
    w_up_p = np.concatenate(cols, axis=1)
    par = np.zeros((128, NPAR), np.float32)
    par[:, 0:8] = _vec(f("norm_mix_g")[0])
    par[:, 8:16] = _vec(f("norm_mem_g")[0])
    par[:, 16:24] = _vec(f("mem_kv_norm_g")[0])
    par[:, 24:32] = _vec(f("norm_ffn_g")[0])
    par[:, 32:40] = _vec(f("final_norm_g"))
    par[:, 40:44] = _vec(f("conv_out_g")[0])
    cw = f("conv_mix_w")[0]
    par[:, 44:56] = cw.T.reshape(4, 128, 3).transpose(1, 0, 2).reshape(128, 12)
    fw = f("ffn_conv_w")[0]
    par[:, 56:56 + 66] = fw.T.reshape(NJ, 128, 3).transpose(1, 0, 2).reshape(128, 66)
    cst = np.zeros((128, 320), np.float32)
    cst[:, 0:128] = np.eye(128, dtype=np.float32)
    kk, qq = np.meshgrid(np.arange(128), np.arange(128), indexing="ij")
    cst[:, 128:256] = np.where(kk <= qq, 0.0, NEG).astype(np.float32)
    negm = np.zeros((8, 8), np.float32)
    for qb in range(8):
        for n in range(8):
            negm[qb, n] = 0.0 if n < qb else (1e30 if n == qb else -1e30)
    cst[:, 256:320] = negm.reshape(1, 64)
    return {
        "w_in": _wl(w_in_p), "w_out": _wl(f("w_out_mix")[0]), "w_mq": _wl(f("w_mem_q")[0]),
        "w_mkv": _wl(f("w_mem_kv")[0]), "w_mo": _wl(f("w_mem_o")[0]), "w_up": _wl(w_up_p),
        "w_dn": _wl(f("w_ffn_down")[0]), "par": par,
        "garow": np.ascontiguousarray(f("attn_out_g")[0].reshape(1, 512)), "cst": cst,
    }


_NC_CACHE = {}


def kernel(**inputs):
    n_cores = 8
    x = np.asarray(inputs["x"], dtype=np.float32)
    mem = np.asarray(inputs["mem"], dtype=np.float32)
    Bt = x.shape[0]
    NS = Bt // n_cores
    shared = prep_shared(inputs)
    in_maps = []
    for c in range(n_cores):
        m = dict(shared)
        m["x"] = _fm(x[c * NS:(c + 1) * NS])
        m["mem"] = _fm(mem[c * NS:(c + 1) * NS])
        in_maps.append(m)
    if NS not in _NC_CACHE:
        _NC_CACHE[NS] = build_nc(NS)
    nc = _NC_CACHE[NS]
    res = run_bass_kernel_spmd(nc, in_maps, core_ids=list(range(n_cores)))
    outs = []
    for c in range(n_cores):
        o = res.results[c]["out"]
        o = o.transpose(0, 3, 2, 1).reshape(NS, S_LEN, D)
        outs.append(o)
    return np.ascontiguousarray(np.concatenate(outs, axis=0)).astype(np.float32)
```
